# Optimizing a Trainium2 kernel written in Bass

```python
import jax, jax.numpy as jnp
from jax import lax
import numpy as np

D_MODEL = 2048
BATCH = 4
SEQ = 8192
DEPTH = 1

SGU_CHUNK = 128
SGU_GROUPS = 8
SGU_WIDTH = D_MODEL // 2
SGU_GROUP_DIM = SGU_WIDTH // SGU_GROUPS
GLA_HEADS = 4
GLA_KEY_DIM = D_MODEL // 2
GLA_VAL_DIM = D_MODEL
GLA_HEAD_K = GLA_KEY_DIM // GLA_HEADS
GLA_HEAD_V = GLA_VAL_DIM // GLA_HEADS
GLA_GATE_RANK = 16
GLA_GATE_NORM = 16.0
GLA_CHUNK = 64
N_GROUPS = 8
EXPERTS_PER_GROUP = 8
N_EXPERTS = N_GROUPS * EXPERTS_PER_GROUP
TOP_K = 2
D_EXPERT = D_MODEL // 4
MOE_BLOCK = 128
LN_EPS = 1e-5
DEEPNORM_ALPHA = (2.0 * DEPTH) ** 0.25
DEEPNORM_BETA = (8.0 * DEPTH) ** -0.25
IN_SIZES = (SGU_WIDTH, SGU_WIDTH, GLA_KEY_DIM, GLA_KEY_DIM, GLA_VAL_DIM, GLA_VAL_DIM, GLA_GATE_RANK)
IN_WIDTH = sum(IN_SIZES)

kernel_name = "hybrid_sgu_gla_hmoe_deepnorm"


def layer_norm(x, g, b):
    xf = x.astype(jnp.float32)
    mu = jnp.mean(xf, axis=-1, keepdims=True)
    var = jnp.mean(jnp.square(xf - mu), axis=-1, keepdims=True)
    return ((xf - mu) * lax.rsqrt(var + LN_EPS)).astype(x.dtype) * g + b


def rms_norm(x, g):
    xf = x.astype(jnp.float32)
    return (xf * lax.rsqrt(jnp.mean(jnp.square(xf), axis=-1, keepdims=True) + LN_EPS)).astype(x.dtype) * g


def split_offsets():
    return [sum(IN_SIZES[:i + 1]) for i in range(len(IN_SIZES) - 1)]


def spatial_gating_mixer(u, v, ln_g, ln_b, w_s, b_s):
    B, S, _ = u.shape
    n = S // SGU_CHUNK
    vg = v.reshape(B, S, SGU_GROUPS, SGU_GROUP_DIM)
    vg = layer_norm(vg, ln_g.reshape(SGU_GROUPS, SGU_GROUP_DIM), ln_b.reshape(SGU_GROUPS, SGU_GROUP_DIM))
    vg = vg.reshape(B, n, SGU_CHUNK, SGU_GROUPS, SGU_GROUP_DIM)
    causal = jnp.tril(jnp.ones((SGU_CHUNK, SGU_CHUNK), dtype=bool))
    w = jnp.where(causal[None], w_s, jnp.zeros_like(w_s))
    mixed = jnp.einsum('gij,bnjgd->bnigd', w, vg) + b_s.T[None, None, :, :, None]
    return u * mixed.reshape(B, S, SGU_WIDTH)


def gla_mixer(q, k, v, log_a, g_out, norm_g):
    B, S, _ = q.shape
    n = S // GLA_CHUNK
    C = GLA_CHUNK

    def heads(t, d):
        return t.reshape(B, n, C, GLA_HEADS, d).transpose(0, 3, 1, 2, 4).astype(jnp.float32)

    qh = heads(q, GLA_HEAD_K) * (GLA_HEAD_K ** -0.5)
    kh = heads(k, GLA_HEAD_K)
    vh = heads(v, GLA_HEAD_V)
    b = jnp.cumsum(heads(log_a, GLA_HEAD_K), axis=3)
    b_last = b[:, :, :, -1, :]
    q_dec = qh * jnp.exp(b)
    k_inv = kh * jnp.exp(-b)
    k_to_end = kh * jnp.exp(b_last[:, :, :, None, :] - b)
    causal = jnp.tril(jnp.ones((C, C), dtype=bool))
    attn = jnp.where(causal, jnp.einsum('bhnik,bhnjk->bhnij', q_dec, k_inv), 0.0)
    o_intra = jnp.einsum('bhnij,bhnjv->bhniv', attn, vh)

    def step(state, xs):
        qd, kt, vc, dl = xs
        o = jnp.einsum('bhik,bhkv->bhiv', qd, state)
        state = state * jnp.exp(dl)[..., None] + jnp.einsum('bhjk,bhjv->bhkv', kt, vc)
        return state, o

    xs = (jnp.moveaxis(q_dec, 2, 0), jnp.moveaxis(k_to_end, 2, 0),
          jnp.moveaxis(vh, 2, 0), jnp.moveaxis(b_last, 2, 0))
    state0 = jnp.zeros((B, GLA_HEADS, GLA_HEAD_K, GLA_HEAD_V), jnp.float32)
    _, o_inter = lax.scan(step, state0, xs)
    o = o_intra + jnp.moveaxis(o_inter, 0, 2)
    o = o.transpose(0, 2, 3, 1, 4).reshape(B, S, GLA_HEADS, GLA_HEAD_V)
    o = rms_norm(o, norm_g.reshape(GLA_HEADS, GLA_HEAD_V).astype(jnp.float32))
    return o.reshape(B, S, GLA_VAL_DIM).astype(q.dtype) * jax.nn.silu(g_out)


def mixer_sublayer(x, w_in, w_gate_a2, b_gate_a, sgu_ln_g, sgu_ln_b, sgu_w_s, sgu_b_s,
                   gla_norm_g, w_branch_a, w_branch_b, w_merge, b_merge, w_out):
    h = x @ w_in
    u, v, q, k, vg, g_out, a_lr = jnp.split(h, split_offsets(), axis=-1)
    y_a = spatial_gating_mixer(jax.nn.gelu(u, approximate=False), jax.nn.gelu(v, approximate=False),
                               sgu_ln_g, sgu_ln_b, sgu_w_s, sgu_b_s) @ w_branch_a
    log_a = jax.nn.log_sigmoid((a_lr @ w_gate_a2 + b_gate_a).astype(jnp.float32)) / GLA_GATE_NORM
    y_b = gla_mixer(q, k, vg, log_a, g_out, gla_norm_g) @ w_branch_b
    gates = jax.nn.sigmoid(x @ w_merge + b_merge)
    g_a, g_b = jnp.split(gates, 2, axis=-1)
    return (g_a * y_a + g_b * y_b) @ w_out


def hierarchical_moe(x, w_rg, b_rg, w_re, b_re, w1, w3, w2):
    B, S, D = x.shape
    T = B * S
    xt = x.reshape(T, D)
    g_logits = (xt @ w_rg + b_rg).astype(jnp.float32)
    g_prob = jax.nn.softmax(g_logits, axis=-1)
    g_idx = jnp.argmax(g_logits, axis=-1)
    p_group = jnp.take_along_axis(g_prob, g_idx[:, None], axis=-1)
    e_logits = (xt @ w_re + b_re).astype(jnp.float32).reshape(T, N_GROUPS, EXPERTS_PER_GROUP)
    e_logits = jnp.take_along_axis(e_logits, g_idx[:, None, None], axis=1)[:, 0]
    top_v, top_i = lax.top_k(e_logits, TOP_K)
    weights = p_group * jax.nn.softmax(top_v, axis=-1)
    expert_ids = (g_idx[:, None] * EXPERTS_PER_GROUP + top_i).astype(jnp.int32)

    A = T * TOP_K
    flat_e = expert_ids.reshape(A)
    flat_tok = (jnp.arange(A, dtype=jnp.int32) // TOP_K)
    order = jnp.argsort(flat_e)
    e_sorted = flat_e[order]
    counts = jnp.bincount(flat_e, length=N_EXPERTS)
    starts = jnp.cumsum(counts) - counts
    padded = (counts + MOE_BLOCK - 1) // MOE_BLOCK * MOE_BLOCK
    pad_ends = jnp.cumsum(padded)
    pad_starts = pad_ends - padded
    dest_sorted = pad_starts[e_sorted] + (jnp.arange(A) - starts[e_sorted])
    dest = jnp.zeros((A,), jnp.int32).at[order].set(dest_sorted.astype(jnp.int32))
    P = A + N_EXPERTS * MOE_BLOCK
    n_blocks = P // MOE_BLOCK
    buf_tok = jnp.full((P,), T, jnp.int32).at[dest].set(flat_tok)
    block_expert = jnp.minimum(
        jnp.searchsorted(pad_ends, jnp.arange(n_blocks) * MOE_BLOCK, side='right'), N_EXPERTS - 1)
    x_pad = jnp.concatenate([xt, jnp.zeros((1, D), xt.dtype)], axis=0)

    def expert_block(args):
        tok, e = args
        xb = x_pad[tok]
        return (jax.nn.silu(xb @ w1[e]) * (xb @ w3[e])) @ w2[e]

    y_buf = lax.map(expert_block, (buf_tok.reshape(n_blocks, MOE_BLOCK), block_expert)).reshape(P, D)
    y = jnp.sum(y_buf[dest].reshape(T, TOP_K, D) * weights[..., None].astype(x.dtype), axis=1)
    return y.reshape(B, S, D)


def setup_inputs(seed: int = 0) -> dict:
    key = jax.random.key(seed)
    ks = jax.random.split(key, 32)
    L, D = DEPTH, D_MODEL
    beta = DEEPNORM_BETA

    def nrm(k, shape, scale):
        return jax.random.normal(k, shape, jnp.float32) * scale

    x = nrm(ks[0], (BATCH, SEQ, D), 1.0)
    w_in = jnp.concatenate([
        nrm(ks[1], (L, D, 2 * SGU_WIDTH + 2 * GLA_KEY_DIM), D ** -0.5),
        nrm(ks[2], (L, D, GLA_VAL_DIM), D ** -0.5 * beta),
        nrm(ks[3], (L, D, GLA_VAL_DIM + GLA_GATE_RANK), D ** -0.5),
    ], axis=-1)
    return {
        "x": x,
        "w_in": w_in,
        "w_gate_a2": nrm(ks[4], (L, GLA_GATE_RANK, GLA_KEY_DIM), GLA_GATE_RANK ** -0.5),
        "b_gate_a": nrm(ks[5], (L, GLA_KEY_DIM), 0.1),
        "sgu_ln_g": 1.0 + nrm(ks[6], (L, SGU_WIDTH), 0.02),
        "sgu_ln_b": nrm(ks[7], (L, SGU_WIDTH), 0.02),
        "sgu_w_s": nrm(ks[8], (L, SGU_GROUPS, SGU_CHUNK, SGU_CHUNK), SGU_CHUNK ** -0.5),
        "sgu_b_s": 1.0 + nrm(ks[9], (L, SGU_GROUPS, SGU_CHUNK), 0.02),
        "gla_norm_g": 1.0 + nrm(ks[10], (L, GLA_VAL_DIM), 0.02),
        "w_branch_a": nrm(ks[11], (L, SGU_WIDTH, D), SGU_WIDTH ** -0.5 * beta),
        "w_branch_b": nrm(ks[12], (L, GLA_VAL_DIM, D), GLA_VAL_DIM ** -0.5 * beta),
        "w_merge": nrm(ks[13], (L, D, 2 * D), D ** -0.5),
        "b_merge": nrm(ks[14], (L, 2 * D), 0.02),
        "w_out": nrm(ks[15], (L, D, D), D ** -0.5 * beta),
        "ln1_g": 1.0 + nrm(ks[16], (L, D), 0.02),
        "ln1_b": nrm(ks[17], (L, D), 0.02),
        "w_router_group": nrm(ks[18], (L, D, N_GROUPS), D ** -0.5),
        "b_router_group": nrm(ks[19], (L, N_GROUPS), 0.01),
        "w_router_expert": nrm(ks[20], (L, D, N_EXPERTS), D ** -0.5),
        "b_router_expert": nrm(ks[21], (L, N_EXPERTS), 0.01),
        "w_exp_gate": nrm(ks[22], (L, N_EXPERTS, D, D_EXPERT), D ** -0.5),
        "w_exp_up": nrm(ks[23], (L, N_EXPERTS, D, D_EXPERT), D ** -0.5),
        "w_exp_down": nrm(ks[24], (L, N_EXPERTS, D_EXPERT, D), D_EXPERT ** -0.5 * beta),
        "ln2_g": 1.0 + nrm(ks[25], (L, D), 0.02),
        "ln2_b": nrm(ks[26], (L, D), 0.02),
    }


def reference(x, w_in, w_gate_a2, b_gate_a, sgu_ln_g, sgu_ln_b, sgu_w_s, sgu_b_s, gla_norm_g,
              w_branch_a, w_branch_b, w_merge, b_merge, w_out, ln1_g, ln1_b,
              w_router_group, b_router_group, w_router_expert, b_router_expert,
              w_exp_gate, w_exp_up, w_exp_down, ln2_g, ln2_b):
    h = x
    for l in range(DEPTH):
        mix = mixer_sublayer(h, w_in[l], w_gate_a2[l], b_gate_a[l], sgu_ln_g[l], sgu_ln_b[l],
                             sgu_w_s[l], sgu_b_s[l], gla_norm_g[l], w_branch_a[l], w_branch_b[l],
                             w_merge[l], b_merge[l], w_out[l])
        h = layer_norm(DEEPNORM_ALPHA * h + mix, ln1_g[l], ln1_b[l])
        ffn = hierarchical_moe(h, w_router_group[l], b_router_group[l], w_router_expert[l],
                               b_router_expert[l], w_exp_gate[l], w_exp_up[l], w_exp_down[l])
        h = layer_norm(DEEPNORM_ALPHA * h + ffn, ln2_g[l], ln2_b[l])
    return h
```

```python
import numpy as np
import concourse.bass as bass
import concourse.mybir as mybir
from concourse.bass_utils import run_bass_kernel_spmd
from contextlib import ExitStack

F32 = mybir.dt.float32
BF16 = mybir.dt.bfloat16
I32 = mybir.dt.int32
U32 = mybir.dt.uint32
ALU = mybir.AluOpType
AF = mybir.ActivationFunctionType

D = 2048
NCH = D // 128
IN_W = 8208
C_U, C_V, C_Q, C_K, C_VG, C_G, C_A = 0, 1024, 2048, 3072, 4096, 6144, 8192
NEXP = 64
DEXP = 512
ALPHA = 2.0 ** 0.25
LN_EPS = 1e-5
TB = 256
SLOT = 4096


class Buf:
    __slots__ = ("name", "lw", "rd", "dsem", "dcnt")

    def __init__(self, name):
        self.name = name
        self.lw = {}
        self.rd = {}
        self.dsem = None
        self.dcnt = 0


class Trk:
    EPOCH = 30000

    def __init__(self, nc, same_sync=True):
        self.nc = nc
        self.E = {"pe": nc.tensor, "act": nc.scalar, "dve": nc.vector,
                  "pool": nc.gpsimd, "sp": nc.sync}
        self.cnt = {k: 0 for k in self.E}
        self.sems = {k: [] for k in self.E}
        self.waited = {k: {} for k in self.E}
        self.same_sync = same_sync
        self.dma_bufs = []
        self.nwait = 0

    def _sem(self, e, ep):
        while len(self.sems[e]) <= ep:
            self.sems[e].append(self.nc.alloc_semaphore(name=f"s_{e}_{len(self.sems[e])}"))
        return self.sems[e][ep]

    def _wait(self, e, ev):
        key, sem, val, src = ev
        if src == e and (e == "pe" or not self.same_sync):
            return
        w = self.waited[e]
        if w.get(key, 0) >= val:
            return
        self.E[e].wait_ge(sem, val)
        self.nwait += 1
        w[key] = val

    def _deps(self, e, reads, writes, merge):
        for b in reads:
            for ev in list(b.lw.values()):
                self._wait(e, ev)
        for b in writes:
            if not merge:
                for ev in list(b.lw.values()):
                    self._wait(e, ev)
            for ev in list(b.rd.values()):
                self._wait(e, ev)

    def _record(self, ev, reads, writes, merge):
        for b in reads:
            b.rd[ev[0]] = ev
        for b in writes:
            if merge:
                b.lw[ev[0]] = ev
            else:
                b.lw = {ev[0]: ev}
                b.rd = {}

    def op(self, e, fn, reads=(), writes=(), merge=False):
        self._deps(e, reads, writes, merge)
        ins = fn()
        i = self.cnt[e]
        self.cnt[e] += 1
        ep = i // self.EPOCH
        sem = self._sem(e, ep)
        val = i % self.EPOCH + 1
        ins.then_inc(sem, 1)
        ev = ((e, ep), sem, val, e)
        self._record(ev, reads, writes, merge)

    def dma(self, q, fn, reads=(), writes=(), owner=None, merge=False):
        self._deps(q, reads, writes, merge)
        ins = fn()
        owner = owner or (writes[0] if writes else reads[0])
        if owner.dsem is None:
            owner.dsem = self.nc.alloc_semaphore(name="d_" + owner.name)
            self.dma_bufs.append(owner)
        owner.dcnt += 16
        ins.then_inc(owner.dsem, 16)
        ev = (("dma", owner.name), owner.dsem, owner.dcnt, "dma")
        self._record(ev, reads, writes, merge)

    def barrier(self):
        evs = []
        for e in self.E:
            i = self.cnt[e]
            if i == 0:
                continue
            ep = (i - 1) // self.EPOCH
            evs.append(((e, ep), self.sems[e][ep], (i - 1) % self.EPOCH + 1, e))
        for b in self.dma_bufs:
            evs.append((("dma", b.name), b.dsem, b.dcnt, "dma"))
        for e in self.E:
            for ev in evs:
                if ev[3] == e:
                    continue
                self._wait(e, ev)

    def final_wait(self, e, bufs):
        for b in bufs:
            for ev in list(b.lw.values()):
                self._wait(e, ev)


def build(T, TP, CAP, same_sync=True, do_moe=True):
    assert T % TB == 0 and TP % TB == 0 and CAP % 128 == 0
    NT = T // 128
    nc = bass.Bass("TRN2", target_bir_lowering=False)
    trk = Trk(nc, same_sync)

    def dr(name, shape, dt=F32, kind="ExternalInput"):
        return nc.dram_tensor(name, shape, dt, kind=kind).ap()

    xT_main = dr("xT_main", [D, T])
    xT_pre = dr("xT_pre", [D, TP])
    x_main = dr("x_main", [T, D])
    w_in = dr("w_in", [D, IN_W])
    wg_aug = dr("wg_aug", [17, 1024])
    sgu_g_bc = dr("sgu_g_bc", [128, 1024])
    sgu_b_bc = dr("sgu_b_bc", [128, 1024])
    wsT = dr("wsT", [128, 8, 128])
    bs_bc = dr("bs_bc", [128, 1024])
    ng_bc = dr("ng_bc", [128, D])
    w_ba = dr("w_ba", [1024, D])
    w_bb = dr("w_bb", [D, D])
    w_mg = dr("w_mg", [D, 2 * D])
    bm_fm = dr("bm_fm", [128, 32])
    w_o = dr("w_o", [D, D])
    ln1g_bc = dr("ln1g_bc", [128, D])
    ln1b_bc = dr("ln1b_bc", [128, D])
    w_r = dr("w_r", [D, 72])
    rb_bc = dr("rb_bc", [128, 72])
    w1 = dr("w1", [NEXP, D, DEXP])
    w3 = dr("w3", [NEXP, D, DEXP])
    w2 = dr("w2", [NEXP, DEXP, D])
    ln2g_bc = dr("ln2g_bc", [128, D])
    ln2b_bc = dr("ln2b_bc", [128, D])
    c_identb = dr("c_identb", [128, 128])
    c_identf = dr("c_identf", [128, 128])
    c_triN = dr("c_triN", [128, 128])
    c_mask = dr("c_mask", [128, 8, 128])
    c_lts = dr("c_lts", [128, 128])
    c_iota = dr("c_iota", [128, 64])
    out = dr("out", [T, D], F32, kind="ExternalOutput")
    Xs = dr("Xs", [NEXP * CAP, D], BF16, kind="Internal")
    Ys = dr("Ys", [NEXP * CAP, D], F32, kind="Internal")
    H1 = dr("H1", [T, D], F32, kind="Internal")
    bXs, bYs, bH1, bOut = Buf("Xs"), Buf("Ys"), Buf("H1"), Buf("out")

    stack = [None]

    def sb(name, shape, dt):
        if stack[0] is not None:
            return stack[0].enter_context(nc.sbuf_tensor(name, shape, dt))
        return nc.alloc_sbuf_tensor(name, shape, dt)

    def mm(o, lhsT, rhs, start, stop, reads, writes):
        trk.op("pe", lambda: nc.tensor.matmul(o, lhsT, rhs, start=start, stop=stop),
               reads, writes, merge=not start)

    def tr(o, in_, ident, reads, writes, merge):
        trk.op("pe", lambda: nc.tensor.transpose(o, in_, ident), reads, writes, merge=merge)

    def act(o, in_, func, reads, writes, bias=0.0, scale=1.0, accum=None, merge=False):
        trk.op("act", lambda: nc.scalar.activation(out=o, in_=in_, func=func, bias=bias,
                                                   scale=scale, accum_out=accum),
               reads, writes, merge=merge)

    def tt(o, a, b, op, reads, writes, merge=False, eng="dve"):
        E = nc.vector if eng == "dve" else nc.gpsimd
        trk.op(eng, lambda: E.tensor_tensor(out=o, in0=a, in1=b, op=op), reads, writes, merge=merge)

    def ts(o, a, s1, s2, op0, op1, reads, writes, merge=False, accum=None):
        if op1 is None:
            trk.op("dve", lambda: nc.vector.tensor_scalar(out=o, in0=a, scalar1=s1, scalar2=None,
                                                          op0=op0), reads, writes, merge=merge)
        else:
            trk.op("dve", lambda: nc.vector.tensor_scalar(out=o, in0=a, scalar1=s1, scalar2=s2,
                                                          op0=op0, op1=op1, accum_out=accum),
                   reads, writes, merge=merge)

    def stt(o, a, s, b, op0, op1, reads, writes, merge=False, accum=None):
        trk.op("dve", lambda: nc.vector.scalar_tensor_tensor(out=o, in0=a, scalar=s, in1=b, op0=op0,
                                                             op1=op1, accum_out=accum),
               reads, writes, merge=merge)

    def cp(o, in_, reads, writes, eng="dve", merge=False):
        if eng == "act":
            trk.op("act", lambda: nc.scalar.copy(out=o, in_=in_), reads, writes, merge=merge)
        elif eng == "pool":
            trk.op("pool", lambda: nc.gpsimd.tensor_copy(out=o, in_=in_), reads, writes, merge=merge)
        else:
            trk.op("dve", lambda: nc.vector.tensor_copy(out=o, in_=in_), reads, writes, merge=merge)

    def dma(q, o, in_, reads, writes, owner=None, merge=False):
        E = trk.E[q]
        trk.dma(q, lambda: E.dma_start(out=o, in_=in_), reads, writes, owner=owner, merge=merge)

    pst = nc.alloc_psum_tensor("pst", [128, 8, 512], F32)
    pbank = [Buf(f"ps{i}") for i in range(8)]
    pptr = [0]

    def ps(n=1):
        p = pptr[0]
        if p % n:
            p += n - p % n
        if p + n > 8:
            p = 0
        pptr[0] = (p + n) % 8
        return p, pbank[p:p + n]

    cown = Buf("consts")
    consts = {}

    def const(name, src, shape, dt, q="sp"):
        t = sb("k_" + name, shape, dt)
        b = Buf("c_" + name)
        dma(q, t[:], src, [], [b], owner=cown)
        consts[name] = (t, b)
        return t, b

    identb, b_identb = const("identb", c_identb, [128, 128], BF16, "pool")
    identf, b_identf = const("identf", c_identf, [128, 128], F32)
    triN, b_triN = const("triN", c_triN, [128, 128], F32)
    mask8, b_mask8 = const("mask8", c_mask[:, 0:4, :], [128, 4, 128], BF16, "pool")
    lts, b_lts = const("lts", c_lts, [128, 128], BF16, "pool")
    iota64, b_iota = const("iota", c_iota, [128, 64], F32)
    wga, b_wga = const("wga", wg_aug, [17, 1024], BF16, "pool")
    sgug, b_sgug = const("sgug", sgu_g_bc, [128, 1024], F32)
    sgub, b_sgub = const("sgub", sgu_b_bc, [128, 1024], F32)
    bsbc, b_bsbc = const("bsbc", bs_bc, [128, 1024], F32)
    ngbc, b_ngbc = const("ngbc", ng_bc, [128, D], F32)
    bmfm, b_bmfm = const("bmfm", bm_fm, [128, 32], F32)
    ln1g, b_ln1g = const("ln1g", ln1g_bc, [128, D], F32)
    ln1b, b_ln1b = const("ln1b", ln1b_bc, [128, D], F32)
    wr, b_wr = const("wr", w_r.rearrange("(c p) n -> p c n", p=128), [128, NCH, 72], F32)
    rbbc, b_rbbc = const("rbbc", rb_bc, [128, 72], F32)
    onesb = sb("onesb", [128, 128], BF16)
    b_onesb = Buf("onesb")
    trk.op("dve", lambda: nc.vector.memset(onesb[:], 1.0), [], [b_onesb])
    wsm, b_wsm = const("wsm", wsT, [128, 8, 128], BF16, "pool")
    for hh in range(2):
        tt(wsm[:, hh * 4:(hh + 1) * 4, :], wsm[:, hh * 4:(hh + 1) * 4, :], mask8[:], ALU.mult,
           [b_wsm, b_mask8], [b_wsm])

    S = sb("S", [128, 8, 512], F32)
    Sb = sb("Sb", [128, 8, 512], BF16)
    b_S = [Buf(f"S{i}") for i in range(8)]
    b_Sb = [Buf(f"Sb{i}") for i in range(8)]
    for i in range(8):
        trk.op("dve", lambda i=i: nc.vector.memset(S[:, i, :], 0.0), [], [b_S[i]])
        trk.op("dve", lambda i=i: nc.vector.memset(Sb[:, i, :], 0.0), [], [b_Sb[i]])
    cnt_bc = sb("cnt_bc", [128, 64], F32)
    b_cnt = Buf("cnt")
    trk.op("dve", lambda: nc.vector.memset(cnt_bc[:], 0.0), [], [b_cnt])
    destI = sb("destI", [128, NT, 2], I32)
    wts = sb("wts", [128, NT, 2], F32)
    b_dest = [Buf(f"dest{t}") for t in range(NT)]
    b_wts = [Buf(f"wts{t}") for t in range(NT)]

    NSLOT = 3
    pool_ = {"slots": [sb(f"wslot{i}", [128, SLOT], BF16) for i in range(NSLOT)],
             "bufs": [Buf(f"wslot{i}") for i in range(NSLOT)], "base": 0, "limit": 0}
    plan = []
    issued = [0]
    taken = [0]

    Wp = dr("Wp", [13, 128, SLOT], BF16, kind="Internal")
    Wm = dr("Wm", [73, 128, SLOT], BF16, kind="Internal")
    bWp = [Buf(f"Wp{i}") for i in range(13)]
    bWm = [Buf(f"Wm{i}") for i in range(73)]
    pmode = []

    def issue_next():
        i = issued[0]
        if i >= len(plan):
            return
        src, KC, ncols = plan[i][:3]
        wslot, b_wslot = pool_["slots"], pool_["bufs"]
        s = (i - pool_["base"]) % len(wslot)
        n = KC * ncols
        view = wslot[s][:, 0:n].rearrange("p (k n) -> p k n", k=KC)
        md = pmode[i]
        if md is not None and md[0] == "copy":
            dma("sp", wslot[s][:, 0:n], md[1][md[3], :, 0:n], [md[2][md[3]]], [b_wslot[s]])
        else:
            dma("pool", view, src.rearrange("(k p) n -> p k n", p=128), [], [b_wslot[s]])
            if md is not None:
                dma("sp", md[1][md[3], :, 0:n], wslot[s][:, 0:n], [b_wslot[s]], [md[2][md[3]]],
                    owner=b_wslot[s])
        issued[0] += 1

    def get_slab(src_key):
        i = taken[0]
        assert plan[i][3:] == src_key, (i, plan[i][3:], src_key)
        wslot, b_wslot = pool_["slots"], pool_["bufs"]
        while issued[0] < min(pool_["limit"], i + len(wslot)):
            issue_next()
        taken[0] += 1
        src, KC, ncols = plan[i][:3]
        s = (i - pool_["base"]) % len(wslot)
        view = wslot[s][:, 0:KC * ncols].rearrange("p (k n) -> p k n", k=KC)
        return view, b_wslot[s]

    def plan_add(src, KC, ncols, key, md=None):
        plan.append((src, KC, ncols) + key)
        pmode.append(md)

    def plan_block(prefix, first=False):
        W_, bW_ = (Wp, bWp) if prefix else (Wm, bWm)
        j = [0]

        def md():
            r = ("cast" if first else "copy", W_, bW_, j[0])
            j[0] += 1
            return r
        if prefix:
            cols = [("a", C_A, 16)] + [("k", C_K + i * 256, 256) for i in range(4)] + \
                   [("vg", C_VG + i * 256, 256) for i in range(8)]
        else:
            cols = [("a", C_A, 16)] + [("q", C_Q + i * 256, 256) for i in range(4)] + \
                   [("k", C_K + i * 256, 256) for i in range(4)] + \
                   [("vg", C_VG + i * 256, 256) for i in range(8)] + \
                   [("g", C_G + i * 256, 256) for i in range(8)] + \
                   [("u", C_U + i * 256, 256) for i in range(4)] + \
                   [("v", C_V + i * 256, 256) for i in range(4)]
        for nm, c0, n in cols:
            plan_add(w_in[:, c0:c0 + n], NCH, n, (nm, c0), md())
        if not prefix:
            for p in range(8):
                plan_add(w_mg[:, p * 256:(p + 1) * 256], NCH, 256, ("ga", p), md())
                plan_add(w_ba[:, p * 256:(p + 1) * 256], 8, 256, ("ba", p), md())
                plan_add(w_mg[:, D + p * 256:D + (p + 1) * 256], NCH, 256, ("gb", p), md())
                plan_add(w_bb[:, p * 256:(p + 1) * 256], NCH, 256, ("bb", p), md())
            for p in range(8):
                plan_add(w_o[:, p * 256:(p + 1) * 256], NCH, 256, ("wo", p), md())

    for i_ in range(TP // TB):
        plan_block(True, i_ == 0)
    for i_ in range(T // TB):
        plan_block(False, i_ == 0)
    pool_["limit"] = len(plan)
    if do_moe:
        for e in range(NEXP):
            for h in range(2):
                plan_add(w1[e, :, h * 256:(h + 1) * 256], NCH, 256, ("w1", e, h))
            for h in range(2):
                plan_add(w3[e, :, h * 256:(h + 1) * 256], NCH, 256, ("w3", e, h))
            for h in range(2):
                plan_add(w2[e, :, h * 1024:(h + 1) * 1024], 4, 1024, ("w2", e, h))

    stack[0] = ExitStack()
    xTf = sb("xT", [128, NCH, TB // 2], F32)
    xT = xTf[:].bitcast(BF16)
    b_xT = Buf("xT")
    aaug = sb("aaug", [17, TB], BF16)
    b_aaug = Buf("aaug")
    trk.op("dve", lambda: nc.vector.memset(aaug[:], 1.0), [], [b_aaug])
    bufA = sb("bufA", [128, 8, TB], BF16)
    bufB = sb("bufB", [128, 8, TB], BF16)
    vn = sb("vn", [128, 2, 1024], BF16)
    b_A, b_B, b_vn = Buf("A"), Buf("B"), Buf("vn")
    vg = sb("vg", [128, 2, D], BF16)
    gs = sb("gs", [128, 2, D], BF16)
    b_vg = [Buf("vg0"), Buf("vg1")]
    b_gs = [Buf("gs0"), Buf("gs1")]
    lbuf = sb("lbuf", [128, 1024], F32)
    b_l = Buf("l")
    ebuf = sb("ebuf", [128, 8, 128], F32)
    b_e = Buf("e")
    edl = sb("edl", [128, 8], F32)
    b_edl = Buf("edl")
    qdec = sb("qdec", [128, 8, 128], BF16)
    kinv = sb("kinv", [128, 8, 128], BF16)
    kteT = sb("kteT", [128, 8, 128], BF16)
    kte = sb("kte", [128, 1024], BF16)
    attnT = sb("attnT", [128, 4, 128], BF16)
    b_qdec, b_kinv, b_kteT, b_kte, b_attn = Buf("qdec"), Buf("kinv"), Buf("kteT"), Buf("kte"), Buf("attn")
    ss = sb("ss", [128, 4], F32)
    rinv = sb("rinv", [128, 4], F32)
    b_ss, b_rinv = Buf("ss"), Buf("rinv")
    junk = sb("junk", [128, 512], BF16)
    b_junk = Buf("junk")
    og = sb("og", [128, D], BF16)
    b_og = Buf("og")
    ogT = sb("ogT", [128, NCH, TB], BF16)
    b_ogT = Buf("ogT")
    vt = sb("vt", [128, 256], F32)
    b_vt = Buf("vt")
    vfull = sb("vfull", [128, 2, 1024], F32)
    b_vf = [Buf("vf0"), Buf("vf1")]
    mvall = sb("mvall", [128, 16, 2], F32)
    rstd16 = sb("rstd16", [128, 16], F32)
    b_mvall, b_rstd16 = Buf("mvall"), Buf("rstd16")
    st6 = sb("st6", [128, 4, 6], F32)
    mv = sb("mv", [128, 2], F32)
    rstd = sb("rstd", [128, 1], F32)
    b_st6, b_mv, b_rstd = Buf("st6"), Buf("mv"), Buf("rstd")
    sgt = lbuf
    b_sgt = b_l
    gsb = sb("gsb", [128, 2, TB], F32)
    t1 = sb("t1", [128, 2, TB], F32)
    b_gsb, b_t1 = Buf("gsb"), Buf("t1")
    mT = sb("mT", [128, NCH, TB], BF16)
    b_mT = Buf("mT")
    rbuf = sb("rbuf", [128, 2, D], F32)
    b_r = [Buf("r0"), Buf("r1")]
    h1bf = bufA[:].rearrange("p a b -> p (a b)")
    b_h1bf = b_A
    h1T = xTf
    b_h1T = b_xT
    lg = sb("lg", [128, 72], F32)
    b_lg = Buf("lg")
    sm = sb("sm", [128, 64], F32)
    b_sm = Buf("sm")
    elm = sb("elm", [128, 64], F32)
    top8 = sb("top8", [128, 8], F32)
    idx8 = sb("idx8", [128, 8], U32)
    msk = sb("msk", [128, 2, 64], F32)
    Mb = sb("Mb", [128, 64], BF16)
    posf = sb("posf", [128, 64], F32)
    b_elm, b_top8, b_idx8, b_msk, b_Mb, b_posf = (Buf("elm"), Buf("top8"), Buf("idx8"), Buf("msk"),
                                                  Buf("Mb"), Buf("posf"))

    bc_reg = nc.gpsimd.to_reg(NEXP * CAP - 1)
    def load_xT(src, blk):
        dma("pool", xT, src[:, blk * TB:(blk + 1) * TB].rearrange("(k p) t -> p k t", p=128),
            [], [b_xT])

    def proj_fm(key_name, c0, dst, dst_buf, func, scale=1.0):
        first = True
        for s in range(4):
            slab, bs_ = get_slab((key_name, c0 + s * 256))
            for fc in range(2):
                p, pb = ps(1)
                for kc in range(NCH):
                    mm(pst[:, p, 0:TB], slab[:, kc, fc * 128:(fc + 1) * 128], xT[:, kc, :],
                       kc == 0, kc == NCH - 1, [bs_, b_xT], pb)
                act(dst[:, s * 2 + fc, :], pst[:, p, 0:TB], func, pb, [dst_buf], scale=scale,
                    merge=not first)
                first = False

    def proj_tm(key_name, c0, nslab, evac):
        for s in range(nslab):
            slab, bs_ = get_slab((key_name, c0 + s * 256))
            for t2 in range(2):
                p, pb = ps(1)
                for kc in range(NCH):
                    mm(pst[:, p, 0:256], xT[:, kc, t2 * 128:(t2 + 1) * 128], slab[:, kc, :],
                       kc == 0, kc == NCH - 1, [bs_, b_xT], pb)
                evac(s, t2, pst[:, p, 0:256], pb)

    def proj_a():
        slab, bs_ = get_slab(("a", C_A))
        p, pb = ps(1)
        for kc in range(NCH):
            mm(pst[0:16, p, 0:TB], slab[:, kc, 0:16], xT[:, kc, :], kc == 0, kc == NCH - 1,
               [bs_, b_xT], pb)
        cp(aaug[0:16, :], pst[0:16, p, 0:TB], pb, [b_aaug], eng="act")

    def gla_decay(t2, need_q):
        p, pb = ps(2)
        zp = pst[:, p:p + 2, :]
        for h in range(2):
            mm(pst[:, p + h, :], aaug[0:17, t2 * 128:(t2 + 1) * 128], wga[0:17, h * 512:(h + 1) * 512],
               True, True, [b_aaug, b_wga], [pb[h]])
        lb3 = lbuf[:].rearrange("p (a n) -> p a n", a=2)
        act(lb3, zp, AF.Exp, pb, [b_l], scale=-1.0)
        act(lbuf[:], lbuf[:], AF.Ln, [b_l], [b_l], bias=1.0)
        p2, pb2 = ps(2)
        for kc in range(8):
            mm(pst[:, p2 + kc // 4, (kc % 4) * 128:(kc % 4 + 1) * 128], lbuf[:, kc * 128:(kc + 1) * 128],
               triN[:], True, True, [b_l, b_triN], [pb2[kc // 4]])
        bT = pst[:, p2:p2 + 2, :].rearrange("p a (c i) -> p (a c) i", c=4)
        if need_q:
            act(ebuf[:], bT, AF.Exp, pb2, [b_e])
            tt(qdec[:], bufA[:, :, t2 * 128:(t2 + 1) * 128], ebuf[:], ALU.mult, [b_A, b_e], [b_qdec])
        act(ebuf[:], bT, AF.Exp, pb2, [b_e], scale=-1.0)
        act(edl[:], bT[:, :, 127], AF.Exp, pb2, [b_edl])
        if need_q:
            tt(kinv[:], bufB[:, :, t2 * 128:(t2 + 1) * 128], ebuf[:], ALU.mult, [b_B, b_e], [b_kinv])
        for kc in range(8):
            stt(kteT[:, kc, :], ebuf[:, kc, :], edl[:, kc:kc + 1], bufB[:, kc, t2 * 128:(t2 + 1) * 128],
                ALU.mult, ALU.mult, [b_e, b_edl, b_B], [b_kteT], merge=kc > 0)
        p3, pb3 = ps(1)
        pk = pst[:, p3, :].bitcast(BF16)
        for kc in range(8):
            tr(pk[:, kc * 128:(kc + 1) * 128], kteT[:, kc, :], identb[:], [b_kteT, b_identb], pb3,
               merge=kc > 0)
        cp(kte[:], pk, pb3, [b_kte], eng="act")

    def gla_state(t2):
        for hc in range(8):
            h = hc // 2
            p, pb = ps(1)
            mm(pst[:, p, :], kte[:, hc * 128:(hc + 1) * 128], vg[:, t2, h * 512:(h + 1) * 512],
               True, True, [b_kte, b_vg[t2]], pb)
            stt(S[:, hc, :], S[:, hc, :], edl[:, hc:hc + 1], pst[:, p, :], ALU.mult, ALU.add,
                [b_S[hc], b_edl] + pb, [b_S[hc]])
            cp(Sb[:, hc, :], S[:, hc, :], [b_S[hc]], [b_Sb[hc]], eng="act")

    def gla_out(t2):
        p, pb = ps(1)
        ap4 = pst[:, p, :].rearrange("p (h i) -> p h i", h=4)
        for h in range(4):
            for c in range(2):
                mm(ap4[:, h, :], kinv[:, 2 * h + c, :], qdec[:, 2 * h + c, :], c == 0, c == 1,
                   [b_kinv, b_qdec], pb)
        tt(attnT[:], ap4, mask8[:], ALU.mult, pb + [b_mask8], [b_attn])
        p4, pb4 = ps(4)
        for h in range(4):
            mm(pst[:, p4 + h, :], attnT[:, h, :], vg[:, t2, h * 512:(h + 1) * 512], True, False,
               [b_attn, b_vg[t2]], [pb4[h]])
            for c in range(2):
                mm(pst[:, p4 + h, :], qdec[:, 2 * h + c, :], Sb[:, 2 * h + c, :], False, c == 1,
                   [b_qdec, b_Sb[2 * h + c]], [pb4[h]])
        for h in range(4):
            act(junk[:], pst[:, p4 + h, :], AF.Square, [pb4[h]], [b_junk, b_ss], accum=ss[:, h:h + 1],
                merge=h > 0)
        act(rinv[:], ss[:], AF.Ln, [b_ss], [b_rinv], bias=LN_EPS, scale=1.0 / 512.0)
        act(rinv[:], rinv[:], AF.Exp, [b_rinv], [b_rinv], scale=-0.5)
        for h in range(4):
            stt(og[:, h * 512:(h + 1) * 512], pst[:, p4 + h, :], rinv[:, h:h + 1],
                gs[:, t2, h * 512:(h + 1) * 512], ALU.mult, ALU.mult,
                [pb4[h], b_rinv, b_gs[t2]], [b_og], merge=h > 0)

    def gla_ogT(t2):
        for half in range(2):
            p, pb = ps(1)
            pk = pst[:, p, :].bitcast(BF16)
            for c in range(8):
                vc = half * 8 + c
                tr(pk[:, c * 128:(c + 1) * 128], og[:, vc * 128:(vc + 1) * 128], identb[:],
                   [b_og, b_identb], pb, merge=c > 0)
            cp(ogT[:, half * 8:(half + 1) * 8, t2 * 128:(t2 + 1) * 128],
               pk.rearrange("p (c i) -> p c i", c=8), pb, [b_ogT], eng="act",
               merge=not (t2 == 0 and half == 0))

    def evac_vg(s, t2, pp, pb):
        cp(vg[:, t2, s * 256:(s + 1) * 256], pp, pb, [b_vg[t2]], eng="dve", merge=s > 0)

    def evac_g(s, t2, pp, pb):
        act(vt[:], pp, AF.Silu, pb, [b_vt])
        tt(gs[:, t2, s * 256:(s + 1) * 256], vt[:], ngbc[:, s * 256:(s + 1) * 256], ALU.mult,
           [b_vt, b_ngbc], [b_gs[t2]], merge=s > 0)

    def evac_v(s, t2, pp, pb):
        act(vfull[:, t2, s * 256:(s + 1) * 256], pp, AF.Gelu, pb, [b_vf[t2]], merge=s > 0)
        for g in range(2):
            idx = t2 * 8 + s * 2 + g
            sl = vfull[:, t2, (s * 2 + g) * 128:(s * 2 + g + 1) * 128]
            trk.op("dve", lambda sl=sl: nc.vector.bn_stats(out=st6[:, 0, :], in_=sl), [b_vf[t2]], [b_st6])
            trk.op("dve", lambda idx=idx: nc.vector.bn_aggr(out=mvall[:, idx, :], in_=st6[:, 0, :]),
                   [b_st6], [b_mvall], merge=not (idx == 0))

    def sgu_norm():
        act(rstd16[:], mvall[:, :, 1], AF.Ln, [b_mvall], [b_rstd16], bias=LN_EPS)
        act(rstd16[:], rstd16[:], AF.Exp, [b_rstd16], [b_rstd16], scale=-0.5)
        for t2 in range(2):
            for g in range(8):
                idx = t2 * 8 + g
                sl = vfull[:, t2, g * 128:(g + 1) * 128]
                ts(sl, sl, mvall[:, idx, 0:1], rstd16[:, idx:idx + 1], ALU.subtract, ALU.mult,
                   [b_vf[t2], b_mvall, b_rstd16], [b_vf[t2]])
            tt(vfull[:, t2, :], vfull[:, t2, :], sgug[:], ALU.mult, [b_vf[t2], b_sgug], [b_vf[t2]])
            tt(vn[:, t2, :], vfull[:, t2, :], sgub[:], ALU.add, [b_vf[t2], b_sgub], [b_vn], merge=t2 > 0)

    def sgu_mix(t2):
        p, pb = ps(2)
        for g in range(8):
            mm(pst[:, p + g // 4, (g % 4) * 128:(g % 4 + 1) * 128], vn[:, t2, g * 128:(g + 1) * 128],
               wsm[:, g, :], True, True, [b_vn, b_wsm], [pb[g // 4]])
        tt(sgt[:].rearrange("p (a n) -> p a n", a=2), pst[:, p:p + 2, :],
           bsbc[:].rearrange("p (a n) -> p a n", a=2), ALU.add, pb + [b_bsbc], [b_sgt])
        tt(bufA[:, :, t2 * 128:(t2 + 1) * 128], sgt[:].rearrange("p (g i) -> p g i", g=8),
           bufA[:, :, t2 * 128:(t2 + 1) * 128], ALU.mult, [b_sgt, b_A], [b_A])

    def merge_phase():
        for pr in range(8):
            slab, bs_ = get_slab(("ga", pr))
            for c in range(2):
                p, pb = ps(1)
                for kc in range(NCH):
                    mm(pst[:, p, 0:TB], slab[:, kc, c * 128:(c + 1) * 128], xT[:, kc, :], kc == 0,
                       kc == NCH - 1, [bs_, b_xT], pb)
                act(gsb[:, c, :], pst[:, p, 0:TB], AF.Sigmoid, pb + [b_bmfm], [b_gsb],
                    bias=bmfm[:, pr * 2 + c:pr * 2 + c + 1], merge=c > 0)
            slab, bs_ = get_slab(("ba", pr))
            for c in range(2):
                p, pb = ps(1)
                for kc in range(8):
                    mm(pst[:, p, 0:TB], slab[:, kc, c * 128:(c + 1) * 128], bufA[:, kc, :], kc == 0,
                       kc == 7, [bs_, b_A], pb)
                tt(t1[:, c, :], gsb[:, c, :], pst[:, p, 0:TB], ALU.mult, pb + [b_gsb], [b_t1], merge=c > 0)
            slab, bs_ = get_slab(("gb", pr))
            for c in range(2):
                p, pb = ps(1)
                for kc in range(NCH):
                    mm(pst[:, p, 0:TB], slab[:, kc, c * 128:(c + 1) * 128], xT[:, kc, :], kc == 0,
                       kc == NCH - 1, [bs_, b_xT], pb)
                act(gsb[:, c, :], pst[:, p, 0:TB], AF.Sigmoid, pb + [b_bmfm], [b_gsb],
                    bias=bmfm[:, 16 + pr * 2 + c:16 + pr * 2 + c + 1], merge=c > 0)
            slab, bs_ = get_slab(("bb", pr))
            for c in range(2):
                p, pb = ps(1)
                for kc in range(NCH):
                    mm(pst[:, p, 0:TB], slab[:, kc, c * 128:(c + 1) * 128], ogT[:, kc, :], kc == 0,
                       kc == NCH - 1, [bs_, b_ogT], pb)
                tt(gsb[:, c, :], gsb[:, c, :], pst[:, p, 0:TB], ALU.mult, pb + [b_gsb], [b_gsb])
                tt(mT[:, pr * 2 + c, :], gsb[:, c, :], t1[:, c, :], ALU.add, [b_gsb, b_t1], [b_mT],
                   merge=not (pr == 0 and c == 0))

    def wout_phase(blk):
        for t2 in range(2):
            tok0 = blk * TB + t2 * 128
            dma("pool", rbuf[:, t2, :], x_main[tok0:tok0 + 128, :], [], [b_r[t2]])
        for s in range(8):
            slab, bs_ = get_slab(("wo", s))
            for t2 in range(2):
                p, pb = ps(1)
                for kc in range(NCH):
                    mm(pst[:, p, 0:256], mT[:, kc, t2 * 128:(t2 + 1) * 128], slab[:, kc, :], kc == 0,
                       kc == NCH - 1, [bs_, b_mT], pb)
                sl = rbuf[:, t2, s * 256:(s + 1) * 256]
                stt(sl, sl, ALPHA, pst[:, p, 0:256], ALU.mult, ALU.add, pb + [b_r[t2]], [b_r[t2]])

    def layer_norm(xap, xb, gt, gb_, bt, bb_):
        for q in range(4):
            trk.op("dve", lambda q=q: nc.vector.bn_stats(out=st6[:, q, :], in_=xap[:, q * 512:(q + 1) * 512]),
                   [xb], [b_st6], merge=q > 0)
        trk.op("dve", lambda: nc.vector.bn_aggr(out=mv[:], in_=st6[:].rearrange("p a b -> p (a b)")),
               [b_st6], [b_mv])
        act(rstd[:], mv[:, 1:2], AF.Ln, [b_mv], [b_rstd], bias=LN_EPS)
        act(rstd[:], rstd[:], AF.Exp, [b_rstd], [b_rstd], scale=-0.5)
        ts(xap, xap, mv[:, 0:1], rstd[:, 0:1], ALU.subtract, ALU.mult, [xb, b_mv, b_rstd], [xb])
        tt(xap, xap, gt[:], ALU.mult, [xb, gb_], [xb])
        tt(xap, xap, bt[:], ALU.add, [xb, bb_], [xb])

    def route(blk, t2):
        t = blk * 2 + t2
        tok0 = t * 128
        h1 = rbuf[:, t2, :]
        hb = b_r[t2]
        layer_norm(h1, hb, ln1g, b_ln1g, ln1b, b_ln1b)
        dma("pool", H1[tok0:tok0 + 128, :], h1, [hb], [bH1], owner=hb, merge=True)
        cp(h1bf, h1, [hb], [b_h1bf], eng="act")
        for q in range(4):
            p, pb = ps(1)
            for c in range(4):
                dc = q * 4 + c
                tr(pst[:, p, c * 128:(c + 1) * 128], h1[:, dc * 128:(dc + 1) * 128], identf[:],
                   [hb, b_identf], pb, merge=c > 0)
            cp(h1T[:, q * 4:(q + 1) * 4, :], pst[:, p, :].rearrange("p (c i) -> p c i", c=4), pb, [b_h1T],
               eng="act", merge=q > 0)
        p, pb = ps(1)
        for dc in range(NCH):
            mm(pst[:, p, 0:72], h1T[:, dc, :], wr[:, dc, :], dc == 0, dc == NCH - 1, [b_h1T, b_wr], pb)
        tt(lg[:], pst[:, p, 0:72], rbbc[:], ALU.add, pb + [b_rbbc], [b_lg])
        trk.op("dve", lambda: nc.vector.max(out=top8[:], in_=lg[:, 0:8]), [b_lg], [b_top8])
        ts(sm[:, 0:8], lg[:, 0:8], top8[:, 0:1], None, ALU.is_equal, None, [b_lg, b_top8], [b_sm])
        ts(sm[:, 8:9], top8[:, 0:1], -1.0, None, ALU.mult, None, [b_top8], [b_sm], merge=True)
        act(sm[:, 16:24], lg[:, 0:8], AF.Exp, [b_lg, b_sm], [b_sm], bias=sm[:, 8:9], accum=sm[:, 9:10],
            merge=True)
        trk.op("dve", lambda: nc.vector.reciprocal(out=sm[:, 10:11], in_=sm[:, 9:10]), [b_sm], [b_sm])
        ts(sm[:, 24:32], sm[:, 0:8], 1.0, 1e30, ALU.subtract, ALU.mult, [b_sm], [b_sm])
        for g in range(8):
            ts(elm[:, g * 8:(g + 1) * 8], lg[:, 8 + g * 8:16 + g * 8], sm[:, 24 + g:25 + g], None, ALU.add,
               None, [b_lg, b_sm], [b_elm], merge=g > 0)
        trk.op("dve", lambda: nc.vector.max(out=top8[:], in_=elm[:]), [b_elm], [b_top8])
        trk.op("dve", lambda: nc.vector.max_index(out=idx8[:], in_max=top8[:], in_values=elm[:]),
               [b_elm, b_top8], [b_idx8])
        tt(sm[:, 11:12], top8[:, 1:2], top8[:, 0:1], ALU.subtract, [b_top8], [b_sm])
        act(sm[:, 12:13], sm[:, 11:12], AF.Exp, [b_sm], [b_sm])
        ts(sm[:, 13:14], sm[:, 12:13], 1.0, None, ALU.add, None, [b_sm], [b_sm])
        trk.op("dve", lambda: nc.vector.reciprocal(out=sm[:, 14:15], in_=sm[:, 13:14]), [b_sm], [b_sm])
        tt(wts[:, t, 0:1], sm[:, 14:15], sm[:, 10:11], ALU.mult, [b_sm], [b_wts[t]])
        stt(wts[:, t, 1:2], sm[:, 12:13], sm[:, 14:15], sm[:, 10:11], ALU.mult, ALU.mult, [b_sm], [b_wts[t]])
        cp(sm[:, 32:34], idx8[:, 0:2], [b_idx8], [b_sm])
        for k in range(2):
            ts(msk[:, k, :], iota64[:], sm[:, 32 + k:33 + k], None, ALU.is_equal, None, [b_iota, b_sm],
               [b_msk], merge=k > 0)
        tt(Mb[:], msk[:, 0, :], msk[:, 1, :], ALU.add, [b_msk], [b_Mb])
        p, pb = ps(1)
        mm(pst[:, p, 0:64], lts[:], Mb[:], True, True, [b_lts, b_Mb], pb)
        mm(pst[:, p, 64:128], onesb[:], Mb[:], True, True, [b_onesb, b_Mb], pb)
        tt(posf[:], pst[:, p, 0:64], cnt_bc[:], ALU.add, pb + [b_cnt], [b_posf])
        tt(cnt_bc[:], cnt_bc[:], pst[:, p, 64:128], ALU.add, pb + [b_cnt], [b_cnt])
        for k in range(2):
            stt(elm[:], posf[:], 1.0, msk[:, k, :], ALU.mult, ALU.mult, [b_posf, b_msk], [b_elm, b_sm],
                accum=sm[:, 34 + k:35 + k])
            ts(sm[:, 36 + k:37 + k], sm[:, 34 + k:35 + k], float(CAP), 1e7, ALU.is_ge, ALU.mult, [b_sm], [b_sm])
            stt(sm[:, 38 + k:39 + k], sm[:, 32 + k:33 + k], float(CAP), sm[:, 34 + k:35 + k], ALU.mult,
                ALU.add, [b_sm], [b_sm])
            tt(sm[:, 38 + k:39 + k], sm[:, 38 + k:39 + k], sm[:, 36 + k:37 + k], ALU.add, [b_sm], [b_sm])
        cp(destI[:, t, :], sm[:, 38:40], [b_sm], [b_dest[t]])
        if do_moe:
            for k in range(2):
                trk.dma("pool", lambda k=k: nc.gpsimd.indirect_dma_start(
                    out=Xs, out_offset=bass.IndirectOffsetOnAxis(ap=destI[:, t, k:k + 1], axis=0),
                    in_=h1bf, in_offset=None, bounds_check=bc_reg, oob_is_err=False),
                    [b_h1bf, b_dest[t]], [bXs], owner=b_h1bf, merge=True)

    for blk in range(TP // TB):
        load_xT(xT_pre, blk)
        proj_a()
        proj_fm("k", C_K, bufB, b_B, AF.Copy)
        proj_tm("vg", C_VG, 8, evac_vg)
        for t2 in range(2):
            gla_decay(t2, False)
            gla_state(t2)

    for blk in range(T // TB):
        load_xT(xT_main, blk)
        proj_a()
        proj_fm("q", C_Q, bufA, b_A, AF.Copy, scale=1.0 / 16.0)
        proj_fm("k", C_K, bufB, b_B, AF.Copy)
        proj_tm("vg", C_VG, 8, evac_vg)
        proj_tm("g", C_G, 8, evac_g)
        for t2 in range(2):
            gla_decay(t2, True)
            gla_out(t2)
            gla_state(t2)
            gla_ogT(t2)
        proj_fm("u", C_U, bufA, b_A, AF.Gelu)
        proj_tm("v", C_V, 4, evac_v)
        sgu_norm()
        for t2 in range(2):
            sgu_mix(t2)
        merge_phase()
        wout_phase(blk)
        for t2 in range(2):
            route(blk, t2)

    if not do_moe:
        trk.barrier()
        for t in range(NT):
            dma("sp", rbuf[:, 0, :], H1[t * 128:(t + 1) * 128, :], [bH1], [b_r[0]])
            dma("sp", out[t * 128:(t + 1) * 128, :], rbuf[:, 0, :], [b_r[0]], [bOut], owner=b_r[0], merge=True)
        trk.final_wait("sp", [bOut])
        return nc

    trk.barrier()
    stack[0].close()
    stack[0] = ExitStack()
    NST = CAP // 128
    NSM = 2
    pool_["slots"] = pool_["slots"] + [sb(f"mslot{i}", [128, SLOT], BF16) for i in range(NSM)]
    pool_["bufs"] = pool_["bufs"] + [Buf(f"mslot{i}") for i in range(NSM)]
    pool_["base"] = taken[0]
    pool_["limit"] = len(plan)
    assert issued[0] == taken[0]
    xe2 = [sb(f"xe{i}", [128, NST, D], BF16) for i in range(2)]
    b_xe2 = [Buf("xe0"), Buf("xe1")]
    xeT2 = [sb(f"xeT{i}", [128, NCH, CAP], BF16) for i in range(2)]
    b_xeT2 = [Buf("xeT0"), Buf("xeT1")]
    gsil2 = [sb(f"gsil{i}", [128, 4, CAP], F32) for i in range(2)]
    b_gsil2 = [Buf("gsil0"), Buf("gsil1")]
    actT2 = [sb(f"actT{i}", [128, 4, CAP], BF16) for i in range(2)]
    b_actT2 = [Buf("actT0"), Buf("actT1")]
    ye = [sb(f"ye{i}", [128, NST, D], F32) for i in range(2)]
    b_ye = [Buf("ye0"), Buf("ye1")]

    def load_xe(e):
        dma("sp", xe2[e % 2][:], Xs[e * CAP:(e + 1) * CAP, :].rearrange("(s p) d -> p s d", p=128),
            [bXs], [b_xe2[e % 2]])

    load_xe(0)
    for e in range(NEXP):
        if e + 1 < NEXP:
            load_xe(e + 1)
        xe, b_xe = xe2[e % 2], b_xe2[e % 2]
        xeT, b_xeT = xeT2[e % 2], b_xeT2[e % 2]
        gsil, b_gsil = gsil2[e % 2], b_gsil2[e % 2]
        actT, b_actT = actT2[e % 2], b_actT2[e % 2]
        first = True
        for s_ in range(NST):
            for half in range(2):
                p, pb = ps(1)
                pk = pst[:, p, :].bitcast(BF16)
                for c in range(8):
                    dc = half * 8 + c
                    tr(pk[:, c * 128:(c + 1) * 128], xe[:, s_, dc * 128:(dc + 1) * 128], identb[:],
                       [b_xe, b_identb], pb, merge=c > 0)
                cp(xeT[:, half * 8:(half + 1) * 8, s_ * 128:(s_ + 1) * 128],
                   pk.rearrange("p (c i) -> p c i", c=8), pb, [b_xeT], eng="act" if half else "dve",
                   merge=not first)
                first = False
        for h in range(2):
            slab, bs_ = get_slab(("w1", e, h))
            for c in range(2):
                p, pb = ps(1)
                for kc in range(NCH):
                    mm(pst[:, p, 0:CAP], slab[:, kc, c * 128:(c + 1) * 128], xeT[:, kc, :], kc == 0,
                       kc == NCH - 1, [bs_, b_xeT], pb)
                act(gsil[:, h * 2 + c, :], pst[:, p, 0:CAP], AF.Silu, pb, [b_gsil],
                    merge=not (h == 0 and c == 0))
        for h in range(2):
            slab, bs_ = get_slab(("w3", e, h))
            for c in range(2):
                p, pb = ps(1)
                for kc in range(NCH):
                    mm(pst[:, p, 0:CAP], slab[:, kc, c * 128:(c + 1) * 128], xeT[:, kc, :], kc == 0,
                       kc == NCH - 1, [bs_, b_xeT], pb)
                tt(actT[:, h * 2 + c, :], gsil[:, h * 2 + c, :], pst[:, p, 0:CAP], ALU.mult,
                   pb + [b_gsil], [b_actT], merge=not (h == 0 and c == 0))
        yb_ = b_ye[e % 2]
        yt = ye[e % 2]
        first = True
        for h in range(2):
            slab, bs_ = get_slab(("w2", e, h))
            for s_ in range(NST):
                for n in range(2):
                    p, pb = ps(1)
                    for kc in range(4):
                        mm(pst[:, p, :], actT[:, kc, s_ * 128:(s_ + 1) * 128], slab[:, kc, n * 512:(n + 1) * 512],
                           kc == 0, kc == 3, [bs_, b_actT], pb)
                    cp(yt[:, s_, h * 1024 + n * 512:h * 1024 + (n + 1) * 512], pst[:, p, :], pb, [yb_],
                       eng="act" if n else "dve", merge=not first)
                    first = False
        dma("sp", Ys[e * CAP:(e + 1) * CAP, :].rearrange("(s p) d -> p s d", p=128), yt[:], [yb_], [bYs],
            owner=yb_, merge=True)

    trk.barrier()
    stack[0].close()
    stack[0] = ExitStack()
    rbuf = sb("rbuf2", [128, 2, D], F32)
    st6 = sb("st6b", [128, 4, 6], F32)
    mv = sb("mvb", [128, 2], F32)
    rstd = sb("rstdb", [128, 1], F32)
    ln2g, b_ln2g = const("ln2g", ln2g_bc, [128, D], F32)
    ln2b, b_ln2b = const("ln2b", ln2b_bc, [128, D], F32)
    y0 = [sb(f"y0_{i}", [128, D], F32) for i in range(2)]
    y1 = [sb(f"y1_{i}", [128, D], F32) for i in range(2)]
    b_y0 = [Buf("y0_0"), Buf("y0_1")]
    b_y1 = [Buf("y1_0"), Buf("y1_1")]
    for i in range(2):
        trk.op("dve", lambda i=i: nc.vector.memset(y0[i][:], 0.0), [], [b_y0[i]])
        trk.op("dve", lambda i=i: nc.vector.memset(y1[i][:], 0.0), [], [b_y1[i]])
    for t in range(NT):
        i = t % 2
        hb = b_r[i]
        h1 = rbuf[:, i, :]
        dma("sp", h1, H1[t * 128:(t + 1) * 128, :], [bH1], [hb])
        for k, (yt, yb_) in enumerate(((y0[i], b_y0[i]), (y1[i], b_y1[i]))):
            trk.dma("pool", lambda k=k, yt=yt: nc.gpsimd.indirect_dma_start(
                out=yt[:], out_offset=None, in_=Ys,
                in_offset=bass.IndirectOffsetOnAxis(ap=destI[:, t, k:k + 1], axis=0),
                bounds_check=bc_reg, oob_is_err=False),
                [bYs, b_dest[t]], [yb_])
        act(y0[i][:], y0[i][:], AF.Copy, [b_y0[i], b_wts[t]], [b_y0[i]], scale=wts[:, t, 0:1])
        stt(y0[i][:], y1[i][:], wts[:, t, 1:2], y0[i][:], ALU.mult, ALU.add, [b_y0[i], b_y1[i], b_wts[t]],
            [b_y0[i]])
        stt(h1, h1, ALPHA, y0[i][:], ALU.mult, ALU.add, [hb, b_y0[i]], [hb])
        layer_norm(h1, hb, ln2g, b_ln2g, ln2b, b_ln2b)
        dma("sp", out[t * 128:(t + 1) * 128, :], h1, [hb], [bOut], owner=hb, merge=True)
    trk.final_wait("sp", [bOut])
    assert taken[0] == len(plan), (taken[0], len(plan))
    print("instr counts", trk.cnt, "waits", trk.nwait)
    return nc


def host_consts():
    j = np.arange(128)[:, None]
    i = np.arange(128)[None, :]
    tri = (j <= i).astype(np.float32)
    return {
        "c_identb": np.eye(128, dtype=np.float32),
        "c_identf": np.eye(128, dtype=np.float32),
        "c_triN": (tri * (-1.0 / 16.0)).astype(np.float32),
        "c_mask": np.ascontiguousarray(np.broadcast_to(tri[:, None, :], (128, 8, 128))).astype(np.float32),
        "c_lts": (j < i).astype(np.float32),
        "c_iota": np.ascontiguousarray(np.broadcast_to(np.arange(64, dtype=np.float32)[None, :], (128, 64))),
    }


def bc(v, n=128):
    v = np.asarray(v, np.float32).reshape(1, -1)
    return np.ascontiguousarray(np.broadcast_to(v, (n, v.shape[1])))


def shared_inputs(inp):
    f = lambda a: np.ascontiguousarray(np.asarray(a, np.float32))
    m = {}
    m["w_in"] = f(inp["w_in"][0])
    m["wg_aug"] = f(np.concatenate([inp["w_gate_a2"][0], inp["b_gate_a"][0][None, :]], axis=0))
    m["sgu_g_bc"] = bc(inp["sgu_ln_g"][0])
    m["sgu_b_bc"] = bc(inp["sgu_ln_b"][0])
    m["wsT"] = f(np.transpose(inp["sgu_w_s"][0], (2, 0, 1)))
    m["bs_bc"] = bc(inp["sgu_b_s"][0].reshape(-1))
    m["ng_bc"] = bc(inp["gla_norm_g"][0])
    m["w_ba"] = f(inp["w_branch_a"][0])
    m["w_bb"] = f(inp["w_branch_b"][0])
    m["w_mg"] = f(inp["w_merge"][0])
    m["bm_fm"] = f(inp["b_merge"][0].reshape(32, 128).T)
    m["w_o"] = f(inp["w_out"][0])
    m["ln1g_bc"] = bc(inp["ln1_g"][0])
    m["ln1b_bc"] = bc(inp["ln1_b"][0])
    m["w_r"] = f(np.concatenate([inp["w_router_group"][0], inp["w_router_expert"][0]], axis=1))
    m["rb_bc"] = bc(np.concatenate([inp["b_router_group"][0], inp["b_router_expert"][0]]))
    m["w1"] = f(inp["w_exp_gate"][0])
    m["w3"] = f(inp["w_exp_up"][0])
    m["w2"] = f(inp["w_exp_down"][0])
    m["ln2g_bc"] = bc(inp["ln2_g"][0])
    m["ln2b_bc"] = bc(inp["ln2_b"][0])
    m.update(host_consts())
    return m


CAP_FULL = 256
_NC_CACHE = {}


def kernel(**inputs):
    x = np.asarray(inputs["x"], np.float32)
    B, S, _ = x.shape
    n_cores = 8
    T = S // 2
    shared = shared_inputs(inputs)
    in_maps = []
    for c in range(n_cores):
        b, hf = c // 2, c % 2
        m = dict(shared)
        xm = x[b, hf * T:(hf + 1) * T, :]
        m["x_main"] = np.ascontiguousarray(xm)
        m["xT_main"] = np.ascontiguousarray(xm.T)
        m["xT_pre"] = np.ascontiguousarray(x[b, 0:T, :].T) if hf == 1 else np.zeros((D, T), np.float32)
        in_maps.append(m)
    key = (T, CAP_FULL)
    if key not in _NC_CACHE:
        _NC_CACHE[key] = build(T, T, CAP_FULL)
    nc = _NC_CACHE[key]
    res = run_bass_kernel_spmd(nc, in_maps, core_ids=list(range(n_cores)))
    outp = np.empty((B, S, D), np.float32)
    for c in range(n_cores):
        b, hf = c // 2, c % 2
        outp[b, hf * T:(hf + 1) * T, :] = res.results[c]["out"]
    return outp
```
